# Optimizing a Trainium2 kernel written in Bass

```python
import math
import jax
import jax.numpy as jnp
from jax import lax
import numpy as np

D_MODEL = 4096
BATCH = 2
SEQ = 8192
DEPTH = 4

CTX_LEN = 256
GRID_W = 64
HEAD_DIM = 128
CONV_CH = D_MODEL // 4
CONV_W = 31
NA_W = 3 * D_MODEL // 8
NA_HEADS = NA_W // HEAD_DIM
WIN_R = 8
WIN_C = 16
DIFF_W = 3 * D_MODEL // 8
DIFF_HEADS = DIFF_W // HEAD_DIM
DIFF_DIM = HEAD_DIM // 2
ROPE_BASE = 10000.0
MIX_W = CONV_CH + NA_W + DIFF_W
COL_SIZES = (NA_W, NA_W, DIFF_W, DIFF_W, NA_W, DIFF_W, CONV_CH, CONV_CH)
KV_COLS = 2 * NA_W + 2 * DIFF_W
IN_COLS = KV_COLS + NA_W + DIFF_W + 2 * CONV_CH
N_EXPERTS = 16
EC_CAPACITY = 2
EXPERT_FF = D_MODEL // 8
ADA_RANK = D_MODEL // 16
N_MOD = 6
Q_BLOCK = 128
EPS = 1e-6

kernel_name = 'hybrid_parallel_group_diffusion_block'


def _rms(x):
    xf = x.astype(jnp.float32)
    return (xf * lax.rsqrt(jnp.mean(xf * xf, axis=-1, keepdims=True) + EPS)).astype(x.dtype)


def _layernorm(x, g, b):
    xf = x.astype(jnp.float32)
    mu = jnp.mean(xf, axis=-1, keepdims=True)
    var = jnp.mean(jnp.square(xf - mu), axis=-1, keepdims=True)
    return ((xf - mu) * lax.rsqrt(var + EPS)).astype(x.dtype) * g + b


def _ada(cvec, down, up, bias, n):
    z = jax.nn.silu(cvec) @ down
    m = z @ up[:, :n * D_MODEL] + bias[:n * D_MODEL]
    return m.reshape(cvec.shape[:-1] + (n, D_MODEL))


def _split(p, sizes):
    cuts = [int(s) for s in np.cumsum(sizes)[:-1]]
    return jnp.split(p, cuts, axis=-1)


def _conformer_conv(a_val, a_gate, w_dw, b_dw, ln_g, ln_b, w_pw):
    u = a_val * jax.nn.sigmoid(a_gate)
    u = lax.conv_general_dilated(
        u, w_dw[:, None, :].astype(u.dtype), window_strides=(1,),
        padding=[(CONV_W // 2, CONV_W // 2)],
        dimension_numbers=('NWC', 'WIO', 'NWC'),
        feature_group_count=u.shape[-1]) + b_dw
    u = _layernorm(u, ln_g, ln_b)
    return jax.nn.silu(u) @ w_pw


def _dense_attend(q, k, v):
    s = jnp.einsum('bqhe,bkhe->bhqk', q, k).astype(jnp.float32) * (q.shape[-1] ** -0.5)
    p = jax.nn.softmax(s, axis=-1).astype(v.dtype)
    return jnp.einsum('bhqk,bkhe->bqhe', p, v)


def _na_latent(q, k, v, k_ctx, v_ctx, rpb):
    B, T, H, hd = q.shape
    rows = T // GRID_W
    wr = min(WIN_R, rows)
    nqb = GRID_W // WIN_C
    kw = 2 * WIN_C
    qcol = np.arange(GRID_W).reshape(nqb, WIN_C)
    kc0 = np.clip(np.arange(nqb) * WIN_C - WIN_C // 2, 0, GRID_W - kw)
    kcol = kc0[:, None] + np.arange(kw)
    cs = np.clip(qcol - WIN_C // 2, 0, GRID_W - WIN_C)
    col_ok = (kcol[:, None, :] >= cs[:, :, None]) & (kcol[:, None, :] < cs[:, :, None] + WIN_C)
    dc_idx = np.clip(kcol[:, None, :] - qcol[:, :, None] + WIN_C - 1, 0, 2 * WIN_C - 2)
    col_ok = jnp.asarray(col_ok)[:, :, None, :]
    rpb_c = rpb.astype(jnp.float32)[:, :, dc_idx]
    qg = q.reshape(B, rows, nqb, WIN_C, H, hd)
    kg = k.reshape(B, rows, GRID_W, H, hd)
    vg = v.reshape(B, rows, GRID_W, H, hd)
    scale = hd ** -0.5

    def row_fn(r):
        rs = jnp.clip(r - wr // 2, 0, rows - wr)
        ks = lax.dynamic_slice_in_dim(kg, rs, wr, axis=1)[:, :, kcol]
        vs = lax.dynamic_slice_in_dim(vg, rs, wr, axis=1)[:, :, kcol]
        qr = lax.dynamic_index_in_dim(qg, r, axis=1, keepdims=False)
        s_loc = jnp.einsum('bjqhd,brjkhd->bhjqrk', qr, ks).astype(jnp.float32) * scale
        dr_idx = rs + jnp.arange(wr) - r + WIN_R - 1
        bias = jnp.take(rpb_c, dr_idx, axis=1).transpose(0, 2, 3, 1, 4)
        s_loc = jnp.where(col_ok, s_loc + bias, -jnp.inf)
        s_ctx = jnp.einsum('bjqhd,bkhd->bhjqk', qr, k_ctx).astype(jnp.float32) * scale
        s = jnp.concatenate([s_loc.reshape(B, H, nqb, WIN_C, wr * kw), s_ctx], axis=-1)
        p = jax.nn.softmax(s, axis=-1).astype(v.dtype)
        p_loc = p[..., :wr * kw].reshape(B, H, nqb, WIN_C, wr, kw)
        p_ctx = p[..., wr * kw:]
        o = (jnp.einsum('bhjqrk,brjkhd->bjqhd', p_loc, vs)
             + jnp.einsum('bhjqk,bkhd->bjqhd', p_ctx, v_ctx))
        return o.reshape(B, GRID_W, H * hd)

    o = lax.map(row_fn, jnp.arange(rows))
    return jnp.moveaxis(o, 0, 1).reshape(B, T, H * hd)


def _rope_axial(t, cos_r, sin_r, cos_c, sin_c):
    tf = t.astype(jnp.float32)
    half = t.shape[-1] // 2

    def rot(u, cos, sin):
        u1, u2 = jnp.split(u, 2, axis=-1)
        cos = cos[None, :, None, None, :]
        sin = sin[None, :, None, None, :]
        return jnp.concatenate([u1 * cos - u2 * sin, u2 * cos + u1 * sin], axis=-1)

    out = jnp.concatenate([rot(tf[..., :half], cos_r, sin_r), rot(tf[..., half:], cos_c, sin_c)], axis=-1)
    return out.astype(t.dtype)


def _diff_attend(q, k, v, lam):
    s = jnp.einsum('bqhcd,bkhcd->bhcqk', q, k).astype(jnp.float32) * (q.shape[-1] ** -0.5)
    p = jax.nn.softmax(s, axis=-1)
    a = (p[:, :, 0] - lam * p[:, :, 1]).astype(v.dtype)
    return jnp.einsum('bhqk,bkhe->bqhe', a, v)


def _diff_latent(q, k, v, k_ctx, v_ctx, lam):
    B, T = q.shape[:2]
    kk = jnp.concatenate([k_ctx, k], axis=1)
    vv = jnp.concatenate([v_ctx, v], axis=1)
    nb = T // Q_BLOCK
    qb = jnp.moveaxis(q.reshape((B, nb, Q_BLOCK) + q.shape[2:]), 1, 0)
    o = lax.map(lambda qi: _diff_attend(qi, kk, vv, lam), qb)
    return jnp.moveaxis(o, 0, 1).reshape((B, T) + v.shape[2:])


def _diff_post(o, g, lam_init):
    B, Q = o.shape[:2]
    return (_rms(o) * g * (1.0 - lam_init)).reshape(B, Q, -1)


def _ec_moe(h, w_router, w_gate, w_up, w_down):
    B, L, D = h.shape
    cap = max(1, EC_CAPACITY * L // N_EXPERTS)
    aff = jax.nn.softmax(jnp.einsum('bld,de->ble', h, w_router).astype(jnp.float32), axis=-1)
    g, idx = lax.top_k(jnp.swapaxes(aff, 1, 2), cap)
    bidx = jnp.arange(B)[:, None, None]
    xs = h[bidx, idx]
    a = jnp.einsum('becd,edf->becf', xs, w_gate)
    u = jnp.einsum('becd,edf->becf', xs, w_up)
    y = jnp.einsum('becf,efd->becd', jax.nn.silu(a) * u, w_down) * g[..., None].astype(h.dtype)
    return jnp.zeros_like(h).at[bidx, idx].add(y)


def setup_inputs(seed: int = 0) -> dict:
    key = jax.random.key(seed)
    ks = jax.random.split(key, 26)
    f32 = jnp.float32
    L, D = DEPTH, D_MODEL

    def nrm(k, shape, s):
        return jax.random.normal(k, shape, f32) * s

    return {
        'x': nrm(ks[0], (BATCH, SEQ, D), 1.0),
        'c': nrm(ks[1], (BATCH, D), 1.0),
        'ctx': nrm(ks[2], (BATCH, CTX_LEN, D), 1.0),
        'c_ctx': nrm(ks[3], (D,), 1.0),
        'ada_down': nrm(ks[4], (L, D, ADA_RANK), D ** -0.5),
        'ada_up': nrm(ks[5], (L, ADA_RANK, N_MOD * D), 0.3 * ADA_RANK ** -0.5),
        'ada_bias': nrm(ks[6], (L, N_MOD * D), 0.02),
        'w_in': nrm(ks[7], (L, D, IN_COLS), D ** -0.5),
        'conv_dw': nrm(ks[8], (L, CONV_W, CONV_CH), CONV_W ** -0.5),
        'conv_db': nrm(ks[9], (L, CONV_CH), 0.02),
        'conv_ln_g': 1.0 + nrm(ks[10], (L, CONV_CH), 0.02),
        'conv_ln_b': nrm(ks[11], (L, CONV_CH), 0.02),
        'conv_pw': nrm(ks[12], (L, CONV_CH, CONV_CH), CONV_CH ** -0.5),
        'na_q_gain': 1.0 + nrm(ks[13], (L, HEAD_DIM), 0.02),
        'na_k_gain': 1.0 + nrm(ks[14], (L, HEAD_DIM), 0.02),
        'na_rpb': nrm(ks[15], (L, NA_HEADS, 2 * WIN_R - 1, 2 * WIN_C - 1), 0.1),
        'diff_q_gain': 1.0 + nrm(ks[16], (L, 2, DIFF_DIM), 0.02),
        'diff_k_gain': 1.0 + nrm(ks[17], (L, 2, DIFF_DIM), 0.02),
        'diff_lam': nrm(ks[18], (L, 4, DIFF_DIM), 0.1),
        'diff_out_gain': 1.0 + nrm(ks[19], (L, HEAD_DIM), 0.02),
        'w_out': nrm(ks[20], (L, MIX_W, D), MIX_W ** -0.5),
        'w_router': nrm(ks[21], (L, D, N_EXPERTS), D ** -0.5),
        'w_gate': nrm(ks[22], (L, N_EXPERTS, D, EXPERT_FF), D ** -0.5),
        'w_up': nrm(ks[23], (L, N_EXPERTS, D, EXPERT_FF), D ** -0.5),
        'w_down': nrm(ks[24], (L, N_EXPERTS, EXPERT_FF, D), EXPERT_FF ** -0.5),
    }


def reference(x, c, ctx, c_ctx, ada_down, ada_up, ada_bias, w_in, conv_dw, conv_db,
              conv_ln_g, conv_ln_b, conv_pw, na_q_gain, na_k_gain, na_rpb, diff_q_gain,
              diff_k_gain, diff_lam, diff_out_gain, w_out, w_router, w_gate, w_up, w_down):
    B, T, _ = x.shape
    Lc = ctx.shape[1]
    nf = DIFF_DIM // 4
    inv = ROPE_BASE ** (-jnp.arange(nf, dtype=jnp.float32) / nf)
    t_idx = jnp.arange(T)
    ang_r = (t_idx // GRID_W).astype(jnp.float32)[:, None] * inv
    ang_c = (t_idx % GRID_W).astype(jnp.float32)[:, None] * inv
    cos_r, sin_r, cos_c, sin_c = jnp.cos(ang_r), jnp.sin(ang_r), jnp.cos(ang_c), jnp.sin(ang_c)

    for l in range(DEPTH):
        update_ctx = l < DEPTH - 1
        lam_init = 0.8 - 0.6 * math.exp(-0.3 * l)
        lv = diff_lam[l].astype(jnp.float32)
        lam = jnp.exp(jnp.sum(lv[0] * lv[1])) - jnp.exp(jnp.sum(lv[2] * lv[3])) + lam_init
        wl = w_in[l]
        m = _ada(c, ada_down[l], ada_up[l], ada_bias[l], N_MOD)
        mc = _ada(c_ctx, ada_down[l], ada_up[l], ada_bias[l], N_MOD if update_ctx else 2)

        hc = _rms(ctx) * (1 + mc[1]) + mc[0]
        kna_c, vna_c, kdf_c, vdf_c = _split(hc @ wl[:, :KV_COLS], COL_SIZES[:4])
        kna_c = _rms(kna_c.reshape(B, Lc, NA_HEADS, HEAD_DIM)) * na_k_gain[l]
        vna_c = vna_c.reshape(B, Lc, NA_HEADS, HEAD_DIM)
        kdf_c = _rms(kdf_c.reshape(B, Lc, DIFF_HEADS, 2, DIFF_DIM)) * diff_k_gain[l]
        vdf_c = vdf_c.reshape(B, Lc, DIFF_HEADS, HEAD_DIM)

        h = _rms(x) * (1 + m[:, 1, None]) + m[:, 0, None]
        k_na, v_na, k_df, v_df, q_na, q_df, a_val, a_gate = _split(h @ wl, COL_SIZES)
        q_na = _rms(q_na.reshape(B, T, NA_HEADS, HEAD_DIM)) * na_q_gain[l]
        k_na = _rms(k_na.reshape(B, T, NA_HEADS, HEAD_DIM)) * na_k_gain[l]
        v_na = v_na.reshape(B, T, NA_HEADS, HEAD_DIM)
        o_na = _na_latent(q_na, k_na, v_na, kna_c, vna_c, na_rpb[l])
        q_df = _rope_axial(_rms(q_df.reshape(B, T, DIFF_HEADS, 2, DIFF_DIM)) * diff_q_gain[l],
                           cos_r, sin_r, cos_c, sin_c)
        k_df = _rope_axial(_rms(k_df.reshape(B, T, DIFF_HEADS, 2, DIFF_DIM)) * diff_k_gain[l],
                           cos_r, sin_r, cos_c, sin_c)
        v_df = v_df.reshape(B, T, DIFF_HEADS, HEAD_DIM)
        o_df = _diff_post(_diff_latent(q_df, k_df, v_df, kdf_c, vdf_c, lam),
                          diff_out_gain[l], lam_init)
        o_cv = _conformer_conv(a_val, a_gate, conv_dw[l], conv_db[l], conv_ln_g[l],
                               conv_ln_b[l], conv_pw[l])
        mix = jnp.concatenate([o_cv, o_na, o_df], axis=-1) @ w_out[l]
        x = x + m[:, 2, None] * mix
        h2 = _rms(x) * (1 + m[:, 4, None]) + m[:, 3, None]
        x = x + m[:, 5, None] * _ec_moe(h2, w_router[l], w_gate[l], w_up[l], w_down[l])

        if update_ctx:
            qna_c, qdf_c, av_c, ag_c = _split(hc @ wl[:, KV_COLS:], COL_SIZES[4:])
            qna_c = _rms(qna_c.reshape(B, Lc, NA_HEADS, HEAD_DIM)) * na_q_gain[l]
            o_na_c = _dense_attend(qna_c, kna_c, vna_c).reshape(B, Lc, NA_W)
            qdf_c = _rms(qdf_c.reshape(B, Lc, DIFF_HEADS, 2, DIFF_DIM)) * diff_q_gain[l]
            o_df_c = _diff_post(_diff_attend(qdf_c, kdf_c, vdf_c, lam), diff_out_gain[l], lam_init)
            o_cv_c = _conformer_conv(av_c, ag_c, conv_dw[l], conv_db[l], conv_ln_g[l],
                                     conv_ln_b[l], conv_pw[l])
            mix_c = jnp.concatenate([o_cv_c, o_na_c, o_df_c], axis=-1) @ w_out[l]
            ctx = ctx + mc[2] * mix_c
            h2c = _rms(ctx) * (1 + mc[4]) + mc[3]
            ctx = ctx + mc[5] * _ec_moe(h2c, w_router[l], w_gate[l], w_up[l], w_down[l])
    return x
```

```python
import math
import numpy as np
import concourse.bass as bass
import concourse.mybir as mybir
from concourse.bass_utils import run_bass_kernel_spmd

F32 = mybir.dt.float32
BF16 = mybir.dt.bfloat16
ALU = mybir.AluOpType
AF = mybir.ActivationFunctionType
EPS = 1e-6
NEG = -30000.0


class Cfg:
    def __init__(s, D=4096, T=8192, LC=256, DEPTH=4, split=1):
        s.D, s.T, s.LC, s.DEPTH, s.split = D, T, LC, DEPTH, split
        s.per_layer = False
        s.KD = D // 128
        s.CONV = D // 4
        s.CC = s.CONV // 128
        s.NAW = 3 * D // 8
        s.NH = s.NAW // 128
        s.COLS = 4 * s.NAW + 2 * s.NAW + 2 * s.CONV
        s.E = 16
        s.FF = D // 8
        s.FC = s.FF // 128
        s.HF = s.E * s.FC
        s.R = D // 16
        s.RP = min(128, s.R)
        s.RC = s.R // s.RP
        s.TL = T // split
        s.NTOK = s.TL + LC
        s.NG = s.TL // 512
        s.capL = max(1, 2 * T // 16)
        s.capC = max(1, 2 * LC // 16)
        s.ROWS = T // 64
        s.RL = s.TL // 64
        n, c = s.NAW, s.CONV
        s.fam = dict(kna=0, vna=n, kdf=2 * n, vdf=3 * n, qna=4 * n, qdf=5 * n, aval=6 * n, agate=6 * n + c)


class Buf:
    __slots__ = ("w", "r", "dsem", "dcnt", "name", "ph")
    _cur = 0

    def __init__(s, name=""):
        s.w = {}
        s.r = {}
        s.dsem = None
        s.dcnt = 0
        s.name = name
        s.ph = Buf._cur


class TR:
    def __init__(s, nc):
        s.nc = nc
        s.eng = {"pe": nc.tensor, "act": nc.scalar, "dve": nc.vector, "pool": nc.gpsimd, "sp": nc.sync}
        s.semof = {}
        s.ccnt = {}
        for k in ("pe", "act", "dve", "pool"):
            s.semof[("c", k)] = nc.alloc_semaphore("c_" + k)
            s.ccnt[k] = 0
        s.waited = {k: {} for k in s.eng}
        s.dbufs = []
        s.nsem = 4
        s.free = []
        s.nslots = 0

    def _deps(s, eng, R, W, fold=False):
        need = {}

        def add(key, val, skip_same):
            if key[0] == "c" and key[1] == eng and (skip_same or eng == "pe"):
                return
            if need.get(key, 0) < val:
                need[key] = val

        for b in R:
            for key, val in b.w.items():
                add(key, val, False)
        for b in W:
            for key, val in b.w.items():
                add(key, val, False)
            for key, val in b.r.items():
                add(key, val, True)
        e = s.eng[eng]
        wd = s.waited[eng]
        pend = []
        for key, val in need.items():
            if wd.get(key, 0) >= val:
                continue
            wd[key] = val
            pend.append((key, val))
        if fold and pend:
            s.folded = pend[0]
            pend = pend[1:]
        else:
            s.folded = None
        for key, val in pend:
            e.wait_ge(s.semof[key], val)

    def _rec(s, key, val, R, W):
        for b in W:
            b.w = {key: val}
            b.r = {}
        for b in R:
            if b.r.get(key, 0) < val:
                b.r[key] = val

    def op(s, eng, f, R=(), W=()):
        s._deps(eng, R, W, fold=True)
        ins = f(s.eng[eng])
        if s.folded is not None:
            ins._wait_ge(s.semof[s.folded[0]], s.folded[1])
        s.ccnt[eng] += 1
        ins.then_inc(s.semof[("c", eng)], 1)
        s._rec(("c", eng), s.ccnt[eng], R, W)
        return ins

    def _dsem(s, b):
        if b.dsem is None:
            if s.free:
                key, total = s.free.pop()
            else:
                key = ("d", s.nslots)
                s.semof[key] = s.nc.alloc_semaphore("d%d" % s.nslots)
                s.nslots += 1
                total = 0
                s.nsem += 1
            b.dsem = key
            b.dcnt = total
            s.dbufs.append(b)
        return b.dsem

    def release_phase(s, ph):
        keep = []
        for b in s.dbufs:
            if b.ph == ph and ph != 0:
                s.free.append((b.dsem, b.dcnt))
                b.dsem = None
            else:
                keep.append(b)
        s.dbufs = keep

    def dma(s, q, out, in_, R=(), W=(), sb=None):
        s._deps(q, R, W)
        b = sb if sb is not None else (W[0] if W else R[0])
        key = s._dsem(b)
        pieces = [(out, in_)]
        osh, ish = tuple(out.shape), tuple(in_.shape)
        if len(osh) == 3 and osh == ish and osh[1] > 16:
            pieces = [(out[:, a:min(a + 16, osh[1]), :], in_[:, a:min(a + 16, osh[1]), :]) for a in range(0, osh[1], 16)]
        for o_, i_ in pieces:
            b.dcnt += 16
            s.eng[q].dma_start(out=o_, in_=i_).then_inc(s.semof[key], 16)
        s._rec(key, b.dcnt, R, W)

    def coll(s, f, R, W):
        s._deps("pool", R, W)
        b = W[0]
        key = s._dsem(b)
        b.dcnt += 16
        f(s.eng["pool"]).then_inc(s.semof[key], 16)
        s._rec(key, b.dcnt, R, W)

    def barrier(s):
        for eng, e in s.eng.items():
            wd = s.waited[eng]
            for k, c in s.ccnt.items():
                key = ("c", k)
                if k != eng and wd.get(key, 0) < c:
                    wd[key] = c
                    e.wait_ge(s.semof[key], c)
            for b in s.dbufs:
                if b.dcnt and wd.get(b.dsem, 0) < b.dcnt:
                    wd[b.dsem] = b.dcnt
                    e.wait_ge(s.semof[b.dsem], b.dcnt)


def build(cfg, nlayers=None, stop_after=None, NSH=8):
    C = cfg
    L = C.DEPTH if nlayers is None else nlayers
    D, KD, TL, LC, NTOK, NH, CC, NAW, CONV = C.D, C.KD, C.TL, C.LC, C.NTOK, C.NH, C.CC, C.NAW, C.CONV
    nc = bass.Bass("TRN2", target_bir_lowering=False)
    Buf._cur = 0
    tr = TR(nc)
    OP, DMA = tr.op, tr.dma

    def din(name, shape, dt=F32):
        return nc.dram_tensor(name, list(shape), dt, kind="ExternalInput")

    def dint(name, shape, dt):
        return nc.dram_tensor(name, list(shape), dt, kind="Internal")

    xT_in = din("xT", [D, NTOK])
    cT_in = din("cT", [128, KD, 2])
    wsh = {}
    WSHAPES = dict(w_in=(D, C.COLS), w_out=(D, D), conv_pw=(CONV, CONV), ada_down=(D, C.R),
                   ada_up=(C.R, 6 * D), w_router=(D, 16), w_gate=(16 * D, C.FF), w_up=(16 * D, C.FF),
                   w_down=(16 * C.FF, D), nab=(NH * 9 * 128, 128))
    for k, (K, N) in WSHAPES.items():
        wsh[k] = din(k, [L, K // NSH, N])
    abias_in = din("abias", [L, 128, 6 * KD])
    convp_in = din("convp", [L, 128, CC, 34])
    gains_in = din("gains", [L, 128, 8])
    lam_in = din("lam", [L, 1, 256])
    rope_in = din("rope", [128, 2, NTOK])
    consts_in = din("consts", [128, 5, 128])
    sel_in = din("sel", [16, 16, 128])
    rmask_in = din("rmask", [128, 5, 9, 128])
    selpn_in = din("selpn", [128, 8])
    outT = nc.dram_tensor("outT", [D, NTOK], F32, kind="ExternalOutput")

    wbf = {k: dint("g_" + k, [K, N], BF16) for k, (K, N) in WSHAPES.items()}
    wsend = {k: dint("s_" + k, [K // NSH, N], BF16) for k, (K, N) in WSHAPES.items()}
    xT = dint("xTs", [D, NTOK], F32)
    qnaT = dint("qnaT", [NAW, NTOK], BF16)
    qdfT = dint("qdfT", [NAW, NTOK], BF16)
    kna_l = dint("kna_l", [NAW, TL], BF16)
    kdf_l = dint("kdf_l", [NAW, TL], BF16)
    vna_l = dint("vna_l", [TL, NAW], BF16)
    vdf_l = dint("vdf_l", [TL, NAW], BF16)
    kna_g = dint("kna_g", [4 * NAW, TL], BF16)
    kdf_g = dint("kdf_g", [4 * NAW, TL], BF16)
    vna_g = dint("vna_g", [4 * TL, NAW], BF16)
    vdf_g = dint("vdf_g", [4 * TL, NAW], BF16)
    kna_c = dint("kna_c", [NAW, LC], BF16)
    kdf_c = dint("kdf_c", [NAW, LC], BF16)
    vna_c = dint("vna_c", [LC, NAW], BF16)
    vdf_c = dint("vdf_c", [LC, NAW], BF16)
    uT_l = dint("uT_l", [CONV, TL], F32)
    uT_g = dint("uT_g", [4 * CONV, TL], F32)
    uT_c = dint("uT_c", [CONV, LC], F32)
    mixT = dint("mixT", [D, NTOK], BF16)
    h2T = dint("h2T", [D, NTOK], BF16)
    aff_l = dint("aff_l", [16, TL], F32)
    aff_g = dint("aff_g", [64, TL], F32)
    aff_c = dint("aff_c", [16, LC], F32)

    GRP4 = [[0, 1, 2, 3], [4, 5, 6, 7]]
    GRP8 = [[0, 1, 2, 3, 4, 5, 6, 7]]

    from contextlib import ExitStack
    stacks = [ExitStack()]
    uid = [0]

    def sb(name, shape, dt):
        uid[0] += 1
        return stacks[-1].enter_context(nc.sbuf_tensor("sb_%s_%d" % (name, uid[0]), list(shape), dt))

    phase_ctr = [0]

    def phase_begin():
        stacks.append(ExitStack())
        phase_ctr[0] += 1
        Buf._cur = phase_ctr[0]

    def phase_end():
        tr.barrier()
        tr.release_phase(Buf._cur)
        Buf._cur = 0
        stacks.pop().close()

    PS = [nc.alloc_psum_tensor("ps%d" % i, [128, 512], F32) for i in range(8)]
    PB = [Buf("ps%d" % i) for i in range(8)]

    cst = sb("cst", [128, 5, 128], F32)
    cstb = sb("cstb", [128, 5, 128], BF16)
    selb_f = sb("selb_f", [16, 16, 128], F32)
    selb = sb("selb", [16, 16, 128], BF16)
    selpn = sb("selpn", [128, 8], F32)
    mT = sb("mT", [128, 6 * KD, 2], F32)
    convp = sb("convp", [128, CC, 34], F32)
    gains = sb("gains", [128, 8], F32)
    lamv = sb("lamv", [128, 2], F32)
    B_c = Buf("consts")
    B_m = Buf("mT")
    B_lp = Buf("layerp")
    for t_sb, t_in in ((cst, consts_in), (selb_f, sel_in), (selpn, selpn_in)):
        DMA("sp", t_sb[:], t_in.ap(), W=[B_c])
    OP("dve", lambda e: e.tensor_copy(out=cstb[:], in_=cst[:]), R=[B_c], W=[B_c])
    OP("dve", lambda e: e.tensor_copy(out=selb[:], in_=selb_f[:]), R=[B_c], W=[B_c])
    ONES_F, ONES_B, BLK_B, ID_B, PERM_B = cst[:, 0, :], cstb[:, 0, :], cstb[:, 1, :], cstb[:, 2, :], cstb[:, 3, :]
    BLK_F = cst[:, 1, :]

    B_x = [Buf("x%d" % g) for g in range(C.NG + 1)]
    groups = [(g * 512, 512) for g in range(C.NG)] + [(TL, LC)]
    for gi, (t0, N) in enumerate(groups):
        for k in range(KD):
            DMA("sp", xT.ap()[k * 128:(k + 1) * 128, t0:t0 + N], xT_in.ap()[k * 128:(k + 1) * 128, t0:t0 + N], W=[B_x[gi]])

    B_w = {k: Buf("w_" + k) for k in WSHAPES}
    B_ws = {k: Buf("ws_" + k) for k in WSHAPES}

    def phase_weights(l):
        phase_begin()
        CH = 8192
        st = [sb("wc_f%d" % i, [128, CH], F32) for i in range(2)]
        so = [sb("wc_b%d" % i, [128, CH], BF16) for i in range(2)]
        Bf = [Buf(), Buf()]
        Bo = [Buf(), Buf()]
        i = 0
        engs = ["dve", "pool", "act"]
        for k, (K, N) in WSHAPES.items():
            tot = (K // NSH) * N
            assert tot % 128 == 0
            per = tot // 128
            src = wsh[k].ap()[l].rearrange("a b -> (a b)").rearrange("(p x) -> p x", p=128)
            dst = (wbf[k] if NSH == 1 else wsend[k]).ap().rearrange("a b -> (a b)").rearrange("(p x) -> p x", p=128)
            for o in range(0, per, CH):
                n = min(CH, per - o)
                j = i % 2
                DMA("sp", st[j][:, 0:n], src[:, o:o + n], W=[Bf[j]])
                en = engs[i % 3]
                if en == "act":
                    OP("act", lambda e: e.copy(out=so[j][:, 0:n], in_=st[j][:, 0:n]), R=[Bf[j]], W=[Bo[j]])
                else:
                    OP(en, lambda e: e.tensor_copy(out=so[j][:, 0:n], in_=st[j][:, 0:n]), R=[Bf[j]], W=[Bo[j]])
                DMA("sp", dst[:, o:o + n], so[j][:, 0:n], R=[Bo[j]], W=[B_w[k] if NSH == 1 else B_ws[k]], sb=Bo[j])
                i += 1
            if NSH == 1:
                pass
            else:
                tr.coll(lambda e: e.collective_compute("AllGather", ALU.bypass, replica_groups=GRP8,
                                                       ins=[wsend[k].ap()], outs=[wbf[k].ap()]),
                        R=[B_ws[k]], W=[B_w[k]])
        phase_end()

    def phase_mod(l):
        phase_begin()
        DMA("sp", convp[:], convp_in.ap()[l], W=[B_lp])
        DMA("sp", gains[:], gains_in.ap()[l], W=[B_lp])
        for col, fac in ((0, 128.0 ** -0.5), (2, 64.0 ** -0.5)):
            OP("dve", lambda e: e.tensor_scalar(out=gains[:, col:col + 1], in0=gains[:, col:col + 1], scalar1=float(fac),
                                                scalar2=None, op0=ALU.mult), R=[B_lp], W=[B_lp])
        OP("dve", lambda e: e.tensor_tensor(out=gains[:, 4:5], in0=gains[:, 4:5], in1=gains[:, 7:8], op=ALU.mult), R=[B_lp], W=[B_lp])
        cT = sb("cT%d" % l, [128, KD, 2], F32)
        sc = sb("sc", [128, KD, 2], BF16)
        ab = sb("ab", [128, 6 * KD], F32)
        ad = sb("ad", [128, KD, C.R], BF16)
        au = sb("au", [128, C.RC, D], BF16)
        zT = sb("zT", [128, C.RC, 2], BF16)
        lv = sb("lv", [1, 256], F32)
        lt = sb("lt", [1, 8], F32)
        Bt = Buf()
        Bz = Buf()
        Bau = Buf()
        DMA("sp", cT[:], cT_in.ap(), W=[Bt])
        DMA("sp", ab[:], abias_in.ap()[l], W=[Bt])
        DMA("sp", ad[:], wbf["ada_down"].ap().rearrange("(k p) r -> p k r", p=128), R=[B_w["ada_down"]], W=[Bt])
        DMA("sp", lv[:], lam_in.ap()[l], W=[Bt])
        OP("act", lambda e: e.activation(out=sc[:], in_=cT[:], func=AF.Silu), R=[Bt], W=[Bz])
        RP = C.RP
        for rc in range(C.RC):
            for k in range(KD):
                OP("pe", lambda e: e.matmul(PS[0][0:RP, rc * 2:rc * 2 + 2], lhsT=ad[:, k, rc * RP:(rc + 1) * RP],
                                            rhs=sc[:, k, :], start=(k == 0), stop=(k == KD - 1)),
                   R=[Bt, Bz], W=[PB[0]])
        OP("dve", lambda e: e.tensor_copy(out=zT[0:RP, :, :], in_=PS[0][0:RP, 0:2 * C.RC].rearrange("p (a b) -> p a b", b=2)),
           R=[PB[0]], W=[Bz])
        aup = wbf["ada_up"].ap().rearrange("(c p) n -> p c n", p=RP)
        for n in range(6):
            DMA("sp", au[0:RP, :, :], aup[:, :, n * D:(n + 1) * D], R=[B_w["ada_up"]], W=[Bau])
            for k in range(KD):
                col = (n * KD + k) * 2
                for rc in range(C.RC):
                    OP("pe", lambda e: e.matmul(PS[1][:, col:col + 2], lhsT=au[0:RP, rc, k * 128:(k + 1) * 128],
                                                rhs=zT[0:RP, rc, :], start=(rc == 0), stop=(rc == C.RC - 1)),
                       R=[Bau, Bz], W=[PB[1]])
        psv = PS[1][:, 0:12 * KD].rearrange("p (a b) -> p a b", b=2)
        for j in range(2):
            OP("dve", lambda e: e.tensor_tensor(out=mT[:, :, j], in0=psv[:, :, j], in1=ab[:], op=ALU.add),
               R=[PB[1], Bt], W=[B_m])
        for n in (1, 4):
            OP("dve", lambda e: e.tensor_scalar(out=mT[:, n * KD:(n + 1) * KD, :], in0=mT[:, n * KD:(n + 1) * KD, :],
                                                scalar1=1.0, scalar2=None, op0=ALU.add), R=[B_m], W=[B_m])
        pr = sb("lpr", [1, 2, 64], F32)
        lv4 = lv[:, :].rearrange("p (a b) -> p a b", b=64)
        OP("dve", lambda e: e.tensor_tensor(out=pr[:, 0, :], in0=lv4[:, 0, :], in1=lv4[:, 1, :], op=ALU.mult), R=[Bt], W=[Bz])
        OP("dve", lambda e: e.tensor_tensor(out=pr[:, 1, :], in0=lv4[:, 2, :], in1=lv4[:, 3, :], op=ALU.mult), R=[Bt, Bz], W=[Bz])
        OP("dve", lambda e: e.reduce_sum(out=lt[:, 0:2], in_=pr[:], axis=mybir.AxisListType.X), R=[Bz], W=[Bz])
        OP("act", lambda e: e.activation(out=lt[:, 2:4], in_=lt[:, 0:2], func=AF.Exp), R=[Bz], W=[Bz])
        OP("dve", lambda e: e.tensor_tensor(out=lt[:, 4:5], in0=lt[:, 3:4], in1=lt[:, 2:3], op=ALU.subtract), R=[Bz], W=[Bz])
        OP("pe", lambda e: e.matmul(PS[2][:, 0:1], lhsT=ONES_F[0:1, :], rhs=lt[:, 4:5], start=True, stop=True),
           R=[Bz, B_c], W=[PB[2]])
        OP("dve", lambda e: e.tensor_tensor(out=lamv[:, 0:1], in0=PS[2][:, 0:1], in1=gains[:, 5:6], op=ALU.subtract),
           R=[PB[2], B_lp], W=[B_lp])
        phase_end()

    def norm_mod(X, HT, N, sidx, n_scale, n_shift, SQ, RS, TMP, Bx, Bh, Bsq, Brs, Btmp, psi):
        for k in range(KD):
            j = k % 2
            OP("act", lambda e: e.activation(out=SQ[j][:, 0:N], in_=X[:, k, 0:N], func=AF.Square), R=[Bx], W=[Bsq[j]])
            OP("pe", lambda e: e.matmul(PS[psi][:, 0:N], lhsT=ONES_F, rhs=SQ[j][:, 0:N], start=(k == 0), stop=(k == KD - 1)),
               R=[Bsq[j], B_c], W=[PB[psi]])
        OP("dve", lambda e: e.tensor_scalar(out=RS[:, 0:N], in0=PS[psi][:, 0:N], scalar1=1.0 / D, scalar2=EPS,
                                            op0=ALU.mult, op1=ALU.add), R=[PB[psi]], W=[Brs])
        OP("act", lambda e: e.sqrt(out=RS[:, 0:N], in_=RS[:, 0:N]), R=[Brs], W=[Brs])
        OP("dve", lambda e: e.reciprocal(out=RS[:, 0:N], in_=RS[:, 0:N]), R=[Brs], W=[Brs])
        for k in range(KD):
            j = k % 2
            OP("dve", lambda e: e.scalar_tensor_tensor(out=TMP[j][:, 0:N], in0=X[:, k, 0:N],
                                                       scalar=mT[:, n_scale * KD + k, sidx:sidx + 1], in1=RS[:, 0:N],
                                                       op0=ALU.mult, op1=ALU.mult), R=[Bx, Brs, B_m], W=[Btmp[j]])
            OP("pool", lambda e: e.tensor_scalar(out=HT[:, k, 0:N], in0=TMP[j][:, 0:N],
                                                 scalar1=mT[:, n_shift * KD + k, sidx:sidx + 1], scalar2=None, op0=ALU.add),
               R=[Btmp[j], B_m], W=[Bh])

    def phase_A(l):
        phase_begin()
        X = sb("A_X", [128, KD, 512], F32)
        HT = sb("A_H", [128, KD, 512], BF16)
        WB = [sb("A_W%d" % i, [128, KD, 256], BF16) for i in range(2)]
        SQ = [sb("A_SQ%d" % i, [128, 512], F32) for i in range(2)]
        ROPE = sb("A_ROPE", [128, 2, 512], F32)
        Brope = Buf()
        TMP = [sb("A_T%d" % i, [128, 512], F32) for i in range(2)]
        RS = sb("A_RS", [128, 512], F32)
        RS2 = [sb("A_RS2%d" % i, [128, 512], F32) for i in range(2)]
        QN = [sb("A_QN%d" % i, [128, 512], F32) for i in range(2)]
        QNB = [sb("A_QNB%d" % i, [128, 512], BF16) for i in range(2)]
        OB = [sb("A_OB%d" % i, [128, 512], BF16) for i in range(3)]
        AV = sb("A_AV", [128, CC, 512], F32)
        UO = [sb("A_UO%d" % i, [128, 512], F32) for i in range(2)]
        Bx, Bh, Brs, Bav = Buf(), Buf(), Buf(), Buf()
        Bw = [Buf(), Buf()]
        Bsq, Bsqb, Btmp, Brs2, Bqn, Bqnb, Buo = ([Buf(), Buf()] for _ in range(7))
        Bob = [Buf(), Buf(), Buf()]
        win = wbf["w_in"].ap().rearrange("(k p) n -> p k n", p=128)
        blocks = []
        for fam in ("kna", "vna", "kdf", "vdf", "qna", "qdf", "aval", "agate"):
            c0 = C.fam[fam]
            wfam = CONV if fam in ("aval", "agate") else NAW
            o = 0
            while o < wfam:
                w = min(256, wfam - o)
                blocks.append((fam, c0 + o, w, o))
                o += w
        cnt = dict(wb=0, ps=0, sq=0, ob=0, qn=0, uo=0)
        B_out = {k: Buf() for k in ("qna", "qdf", "kna", "kdf", "vna", "vdf", "u", "knac", "kdfc", "vnac", "vdfc", "uc")}
        for gi, (t0, N) in enumerate(groups):
            isctx = gi == C.NG
            sidx = 1 if isctx else 0
            DMA("sp", X[:, :, 0:N], xT.ap()[:, t0:t0 + N].rearrange("(k p) t -> p k t", p=128), R=[B_x[gi]], W=[Bx])
            DMA("sp", ROPE[:, :, 0:N], rope_in.ap()[:, :, t0:t0 + N], W=[Brope])
            norm_mod(X, HT, N, sidx, 1, 0, SQ, RS, TMP, Bx, Bh, Bsq, Brs, Btmp, 7)
            for (fam, col0, w, foff) in blocks:
                j = cnt["wb"] % 2
                cnt["wb"] += 1
                DMA("sp", WB[j][:, :, 0:w], win[:, :, col0:col0 + w], R=[B_w["w_in"]], W=[Bw[j]])
                if fam in ("vna", "vdf"):
                    for tt in range(N // 128):
                        pi = cnt["ps"] % 3
                        cnt["ps"] += 1
                        for k in range(KD):
                            OP("pe", lambda e: e.matmul(PS[pi][:, 0:w], lhsT=HT[:, k, tt * 128:(tt + 1) * 128], rhs=WB[j][:, k, 0:w],
                                                        start=(k == 0), stop=(k == KD - 1)), R=[Bh, Bw[j]], W=[PB[pi]])
                        oi = cnt["ob"] % 3
                        cnt["ob"] += 1
                        OP("act", lambda e: e.copy(out=OB[oi][:, 0:w], in_=PS[pi][:, 0:w]), R=[PB[pi]], W=[Bob[oi]])
                        if isctx:
                            dst = (vna_c if fam == "vna" else vdf_c).ap()[tt * 128:(tt + 1) * 128, foff:foff + w]
                            bo = B_out[fam + "c"]
                        else:
                            dst = (vna_l if fam == "vna" else vdf_l).ap()[t0 + tt * 128:t0 + (tt + 1) * 128, foff:foff + w]
                            bo = B_out[fam]
                        DMA("sp", dst, OB[oi][:, 0:w], R=[Bob[oi]], W=[bo], sb=Bob[oi])
                    continue
                for ci in range(w // 128):
                    ch = (foff + ci * 128) // 128
                    pi = cnt["ps"] % 3
                    cnt["ps"] += 1
                    for k in range(KD):
                        OP("pe", lambda e: e.matmul(PS[pi][:, 0:N], lhsT=WB[j][:, k, ci * 128:(ci + 1) * 128], rhs=HT[:, k, 0:N],
                                                    start=(k == 0), stop=(k == KD - 1)), R=[Bh, Bw[j]], W=[PB[pi]])
                    if fam == "aval":
                        OP("act", lambda e: e.copy(out=AV[:, ch, 0:N], in_=PS[pi][:, 0:N]), R=[PB[pi]], W=[Bav])
                        continue
                    if fam == "agate":
                        ui = cnt["uo"] % 2
                        cnt["uo"] += 1
                        OP("act", lambda e: e.activation(out=UO[ui][:, 0:N], in_=PS[pi][:, 0:N], func=AF.Sigmoid),
                           R=[PB[pi]], W=[Buo[ui]])
                        OP("dve", lambda e: e.tensor_tensor(out=UO[ui][:, 0:N], in0=UO[ui][:, 0:N], in1=AV[:, ch, 0:N], op=ALU.mult),
                           R=[Buo[ui], Bav], W=[Buo[ui]])
                        if isctx:
                            DMA("sp", uT_c.ap()[ch * 128:(ch + 1) * 128, :], UO[ui][:, 0:N], R=[Buo[ui]], W=[B_out["uc"]], sb=Buo[ui])
                        else:
                            DMA("sp", uT_l.ap()[ch * 128:(ch + 1) * 128, t0:t0 + N], UO[ui][:, 0:N], R=[Buo[ui]], W=[B_out["u"]], sb=Buo[ui])
                        continue
                    isdf = fam in ("kdf", "qdf")
                    gcol = dict(qna=0, kna=1, qdf=2, kdf=3)[fam]
                    si = cnt["sq"] % 2
                    cnt["sq"] += 1
                    OP("act", lambda e: e.activation(out=SQ[si][:, 0:N], in_=PS[pi][:, 0:N], func=AF.Square), R=[PB[pi]], W=[Bsq[si]])
                    p2 = 3 + si
                    OP("pe", lambda e: e.matmul(PS[p2][:, 0:N], lhsT=(BLK_F if isdf else ONES_F), rhs=SQ[si][:, 0:N], start=True, stop=True),
                       R=[Bsq[si], B_c], W=[PB[p2]])
                    OP("dve", lambda e: e.tensor_scalar(out=RS2[si][:, 0:N], in0=PS[p2][:, 0:N], scalar1=(1.0 / 64 if isdf else 1.0 / 128),
                                                        scalar2=EPS, op0=ALU.mult, op1=ALU.add), R=[PB[p2]], W=[Brs2[si]])
                    OP("act", lambda e: e.sqrt(out=RS2[si][:, 0:N], in_=RS2[si][:, 0:N]), R=[Brs2[si]], W=[Brs2[si]])
                    OP("dve", lambda e: e.reciprocal(out=RS2[si][:, 0:N], in_=RS2[si][:, 0:N]), R=[Brs2[si]], W=[Brs2[si]])
                    oi = cnt["ob"] % 3
                    cnt["ob"] += 1
                    if not isdf:
                        OP("dve", lambda e: e.scalar_tensor_tensor(out=OB[oi][:, 0:N], in0=PS[pi][:, 0:N], scalar=gains[:, gcol:gcol + 1],
                                                                   in1=RS2[si][:, 0:N], op0=ALU.mult, op1=ALU.mult),
                           R=[PB[pi], Brs2[si], B_lp], W=[Bob[oi]])
                    else:
                        qi = cnt["qn"] % 2
                        cnt["qn"] += 1
                        OP("dve", lambda e: e.scalar_tensor_tensor(out=QN[qi][:, 0:N], in0=PS[pi][:, 0:N], scalar=gains[:, gcol:gcol + 1],
                                                                   in1=RS2[si][:, 0:N], op0=ALU.mult, op1=ALU.mult),
                           R=[PB[pi], Brs2[si], B_lp], W=[Bqn[qi]])
                        OP("pool", lambda e: e.tensor_copy(out=QNB[qi][:, 0:N], in_=QN[qi][:, 0:N]), R=[Bqn[qi]], W=[Bqnb[qi]])
                        p3 = 5 + qi
                        OP("pe", lambda e: e.matmul(PS[p3][:, 0:N], lhsT=PERM_B, rhs=QNB[qi][:, 0:N], start=True, stop=True),
                           R=[Bqnb[qi], B_c], W=[PB[p3]])
                        OP("pool", lambda e: e.tensor_tensor(out=QN[qi][:, 0:N], in0=QN[qi][:, 0:N], in1=ROPE[:, 0, 0:N], op=ALU.mult),
                           R=[Bqn[qi], Bqnb[qi], Brope], W=[Bqn[qi]])
                        ti = qi
                        OP("dve", lambda e: e.tensor_tensor(out=TMP[ti][:, 0:N], in0=PS[p3][:, 0:N], in1=ROPE[:, 1, 0:N], op=ALU.mult),
                           R=[PB[p3], Brope], W=[Btmp[ti]])
                        OP("dve", lambda e: e.tensor_tensor(out=OB[oi][:, 0:N], in0=TMP[ti][:, 0:N], in1=QN[qi][:, 0:N], op=ALU.add),
                           R=[Btmp[ti], Bqn[qi]], W=[Bob[oi]])
                    rows = slice(ch * 128, (ch + 1) * 128)
                    if fam in ("qna", "qdf"):
                        dst = (qnaT if fam == "qna" else qdfT).ap()[rows, t0:t0 + N]
                        bo = B_out[fam]
                    elif isctx:
                        dst = (kna_c if fam == "kna" else kdf_c).ap()[rows, :]
                        bo = B_out[fam + "c"]
                    else:
                        dst = (kna_l if fam == "kna" else kdf_l).ap()[rows, t0:t0 + N]
                        bo = B_out[fam]
                    DMA("sp", dst, OB[oi][:, 0:N], R=[Bob[oi]], W=[bo], sb=Bob[oi])
        phase_end()

    RM = sb("RM", [128, 5, 9, 128], BF16)
    THR = sb("THR", [16, 2], F32)
    B_thr = Buf("thr")
    phase_begin()
    rm_f = sb("rm_f", [128, 5, 9, 128], F32)
    DMA("sp", rm_f[:], rmask_in.ap(), W=[B_c])
    OP("dve", lambda e: e.tensor_copy(out=RM[:], in_=rm_f[:]), R=[B_c], W=[B_c])
    phase_end()
    NLT = TL // 128
    NCT = LC // 128

    def rsqrt_ps(dst, Bdst, ps_ap, Bps, scale):
        OP("dve", lambda e: e.tensor_scalar(out=dst, in0=ps_ap, scalar1=scale, scalar2=EPS, op0=ALU.mult, op1=ALU.add), R=[Bps], W=[Bdst])
        OP("act", lambda e: e.sqrt(out=dst, in_=dst), R=[Bdst], W=[Bdst])
        OP("dve", lambda e: e.reciprocal(out=dst, in_=dst), R=[Bdst], W=[Bdst])

    def phase_C(l, do_ctx):
        phase_begin()
        U = sb("C_U", [128, CC, 542], F32)
        A0 = sb("C_A0", [128, 512], F32)
        A1 = sb("C_A1", [128, 512], F32)
        A2 = sb("C_A2", [128, 512], F32)
        Ba2 = Buf()
        CV = sb("C_CV", [128, CC, 512], F32)
        SQ = [sb("C_SQ%d" % i, [128, 512], F32) for i in range(2)]
        ST = sb("C_ST", [128, 4, 512], F32)
        SB_ = sb("C_S", [128, CC, 512], BF16)
        OC = [sb("C_OC%d" % i, [128, 512], BF16) for i in range(2)]
        PW = sb("C_PW", [128, CC, CONV], BF16)
        Bu, Ba0, Ba1, Bcv, Bst, Bs, Bpw = (Buf() for _ in range(7))
        Bsq = [Buf(), Buf()]
        Boc = [Buf(), Buf()]
        Bmix = Buf()
        DMA("sp", PW[:], wbf["conv_pw"].ap().rearrange("(k p) n -> p k n", p=128), R=[B_w["conv_pw"]], W=[Bpw])
        for gi, (t0, N) in enumerate(groups):
            isctx = gi == C.NG
            if isctx and not do_ctx:
                continue
            OP("pool", lambda e: e.memset(U[:, :, 0:15], 0.0), W=[Bu])
            OP("pool", lambda e: e.memset(U[:, :, N + 15:N + 30], 0.0), W=[Bu])
            if isctx:
                DMA("sp", U[:, :, 15:15 + N], uT_c.ap().rearrange("(c p) t -> p c t", p=128), W=[Bu])
            else:
                lo, hi = max(t0 - 15, 0), min(t0 + N + 15, TL)
                DMA("sp", U[:, :, lo - (t0 - 15):hi - (t0 - 15)], uT_l.ap()[:, lo:hi].rearrange("(c p) t -> p c t", p=128), W=[Bu])
            for c in range(CC):
                OP("dve", lambda e: e.tensor_scalar(out=A0[:, 0:N], in0=U[:, c, 0:N], scalar1=convp[:, c, 0:1], scalar2=convp[:, c, 31:32],
                                                    op0=ALU.mult, op1=ALU.add), R=[Bu, B_lp], W=[Ba0])
                for j in range(1, 16):
                    OP("dve", lambda e: e.scalar_tensor_tensor(out=A0[:, 0:N], in0=U[:, c, j:j + N], scalar=convp[:, c, j:j + 1], in1=A0[:, 0:N],
                                                               op0=ALU.mult, op1=ALU.add), R=[Bu, Ba0, B_lp], W=[Ba0])
                OP("pool", lambda e: e.tensor_scalar(out=A1[:, 0:N], in0=U[:, c, 16:16 + N], scalar1=convp[:, c, 16:17], scalar2=None, op0=ALU.mult),
                   R=[Bu, B_lp], W=[Ba1])
                for j in range(17, 31):
                    OP("pool", lambda e: e.tensor_scalar(out=A2[:, 0:N], in0=U[:, c, j:j + N], scalar1=convp[:, c, j:j + 1], scalar2=None, op0=ALU.mult),
                       R=[Bu, B_lp], W=[Ba2])
                    OP("pool", lambda e: e.tensor_tensor(out=A1[:, 0:N], in0=A1[:, 0:N], in1=A2[:, 0:N], op=ALU.add), R=[Ba1, Ba2], W=[Ba1])
                OP("dve", lambda e: e.tensor_tensor(out=CV[:, c, 0:N], in0=A0[:, 0:N], in1=A1[:, 0:N], op=ALU.add), R=[Ba0, Ba1], W=[Bcv])
                OP("pe", lambda e: e.matmul(PS[0][:, 0:N], lhsT=ONES_F, rhs=CV[:, c, 0:N], start=(c == 0), stop=(c == CC - 1)), R=[Bcv, B_c], W=[PB[0]])
                j2 = c % 2
                OP("act", lambda e: e.activation(out=SQ[j2][:, 0:N], in_=CV[:, c, 0:N], func=AF.Square), R=[Bcv], W=[Bsq[j2]])
                OP("pe", lambda e: e.matmul(PS[1][:, 0:N], lhsT=ONES_F, rhs=SQ[j2][:, 0:N], start=(c == 0), stop=(c == CC - 1)), R=[Bsq[j2], B_c], W=[PB[1]])
            MEAN, VAR, TMPc = ST[:, 0, 0:N], ST[:, 1, 0:N], ST[:, 2, 0:N]
            OP("dve", lambda e: e.tensor_scalar(out=MEAN, in0=PS[0][:, 0:N], scalar1=1.0 / CONV, scalar2=None, op0=ALU.mult), R=[PB[0]], W=[Bst])
            OP("dve", lambda e: e.tensor_tensor(out=TMPc, in0=MEAN, in1=MEAN, op=ALU.mult), R=[Bst], W=[Bst])
            OP("dve", lambda e: e.scalar_tensor_tensor(out=VAR, in0=PS[1][:, 0:N], scalar=1.0 / CONV, in1=TMPc, op0=ALU.mult, op1=ALU.subtract),
               R=[PB[1], Bst], W=[Bst])
            OP("dve", lambda e: e.tensor_scalar(out=VAR, in0=VAR, scalar1=EPS, scalar2=None, op0=ALU.add), R=[Bst], W=[Bst])
            OP("act", lambda e: e.sqrt(out=VAR, in_=VAR), R=[Bst], W=[Bst])
            OP("dve", lambda e: e.reciprocal(out=VAR, in_=VAR), R=[Bst], W=[Bst])
            for c in range(CC):
                OP("dve", lambda e: e.tensor_tensor(out=CV[:, c, 0:N], in0=CV[:, c, 0:N], in1=MEAN, op=ALU.subtract), R=[Bcv, Bst], W=[Bcv])
                OP("dve", lambda e: e.tensor_tensor(out=CV[:, c, 0:N], in0=CV[:, c, 0:N], in1=VAR, op=ALU.mult), R=[Bcv, Bst], W=[Bcv])
                OP("act", lambda e: e.activation(out=SB_[:, c, 0:N], in_=CV[:, c, 0:N], func=AF.Silu, scale=convp[:, c, 32:33], bias=convp[:, c, 33:34]),
                   R=[Bcv, B_lp], W=[Bs])
            for co in range(CC):
                pi = 2 + co % 2
                for c in range(CC):
                    OP("pe", lambda e: e.matmul(PS[pi][:, 0:N], lhsT=PW[:, c, co * 128:(co + 1) * 128], rhs=SB_[:, c, 0:N],
                                                start=(c == 0), stop=(c == CC - 1)), R=[Bs, Bpw], W=[PB[pi]])
                oj = co % 2
                OP("act", lambda e: e.copy(out=OC[oj][:, 0:N], in_=PS[pi][:, 0:N]), R=[PB[pi]], W=[Boc[oj]])
                DMA("sp", mixT.ap()[co * 128:(co + 1) * 128, t0:t0 + N], OC[oj][:, 0:N], R=[Boc[oj]], W=[Bmix], sb=Boc[oj])
        phase_end()

    def phase_D(l, do_ctx):
        phase_begin()
        KT = sb("D_KT", [128, TL + 1024 + LC], BF16)
        VT = sb("D_VT", [128, NLT + 8 + NCT, 128], BF16)
        QT = sb("D_QT", [128, NTOK], BF16)
        OH = sb("D_OH", [128, NTOK], BF16)
        NB = sb("D_NB", [128, 9, 128], BF16)
        PT = [sb("D_PT%d" % i, [128, 12, 128], BF16) for i in range(2)]
        RC = [sb("D_RC%d" % i, [128, 128], F32) for i in range(2)]
        Bkt, Bvt, Bqt, Boh, Bnb, Bmix = (Buf() for _ in range(6))
        Bpt = [Buf(), Buf()]
        Brc = [Buf(), Buf()]
        BO = [Buf(), Buf()]
        BS = [Buf(), Buf()]
        npair = NLT
        it = 0
        for h in range(NH):
            rows = slice(h * 128, (h + 1) * 128)
            OP("pool", lambda e: e.memset(KT[:, 0:512], 0.0), W=[Bkt])
            OP("pool", lambda e: e.memset(KT[:, 512 + TL:1024 + TL], 0.0), W=[Bkt])
            OP("pool", lambda e: e.memset(VT[:, 0:4, :], 0.0), W=[Bvt])
            OP("pool", lambda e: e.memset(VT[:, 4 + NLT:8 + NLT, :], 0.0), W=[Bvt])
            DMA("sp", KT[:, 512:512 + TL], kna_l.ap()[rows, :], W=[Bkt])
            DMA("sp", KT[:, 1024 + TL:], kna_c.ap()[rows, :], W=[Bkt])
            DMA("sp", VT[:, 4:4 + NLT, :], vna_l.ap()[:, rows].rearrange("(t p) e -> p t e", p=128), W=[Bvt])
            DMA("sp", VT[:, 8 + NLT:, :], vna_c.ap()[:, rows].rearrange("(t p) e -> p t e", p=128), W=[Bvt])
            DMA("sp", QT[:], qnaT.ap()[rows, :], W=[Bqt])
            DMA("sp", NB[:], wbf["nab"].ap()[h * 9 * 128:(h + 1) * 9 * 128, :].rearrange("(t q) k -> q t k", q=128), R=[B_w["nab"]], W=[Bnb])
            units = [("lat", i) for i in range(npair)] + ([("ctx", j) for j in range(NCT)] if do_ctx else [])
            for kind, i in units:
                s_ = it % 2
                it += 1
                banks = [3 * s_, 3 * s_ + 1, 3 * s_ + 2]
                if kind == "lat":
                    cls = 0 if i == 0 else 1 if i == 1 else 3 if i == npair - 2 else 4 if i == npair - 1 else 2
                    qs = slice(128 * i, 128 * i + 128)
                    tiles = [(KT[:, 128 * (i + t):128 * (i + t) + 128], i + t, t) for t in range(9)]
                    tiles += [(KT[:, 1024 + TL + 128 * j:1024 + TL + 128 * j + 128], 8 + NLT + j, None) for j in range(NCT)]
                else:
                    qs = slice(TL + 128 * i, TL + 128 * i + 128)
                    tiles = [(KT[:, 1024 + TL + 128 * j:1024 + TL + 128 * j + 128], 8 + NLT + j, None) for j in range(NCT)]
                nt = len(tiles)
                for ti, (kap, vidx, tb) in enumerate(tiles):
                    bk = banks[ti // 4]
                    col = (ti % 4) * 128
                    OP("pe", lambda e: e.matmul(PS[bk][:, col:col + 128], lhsT=kap, rhs=QT[:, qs], start=True, stop=(tb is None)),
                       R=[Bkt, Bqt], W=[PB[bk]])
                    if tb is not None:
                        OP("pe", lambda e: e.matmul(PS[bk][:, col:col + 128], lhsT=NB[:, tb, :], rhs=ID_B, start=False, stop=False),
                           R=[Bnb, B_c], W=[PB[bk]])
                        OP("pe", lambda e: e.matmul(PS[bk][:, col:col + 128], lhsT=RM[:, cls, tb, :], rhs=ID_B, start=False, stop=True),
                           R=[B_c], W=[PB[bk]])
                for bi in range((nt + 3) // 4):
                    n_in = min(4, nt - 4 * bi)
                    bk = banks[bi]
                    OP("act", lambda e: e.activation(out=PT[s_][:, 4 * bi:4 * bi + n_in, :], in_=PS[bk][:, 0:128 * n_in].rearrange("p (a b) -> p a b", b=128),
                                                     func=AF.Exp), R=[PB[bk]], W=[Bpt[s_]])
                for ti, (kap, vidx, tb) in enumerate(tiles):
                    OP("pe", lambda e: e.matmul(PS[6][:, 128 * s_:128 * s_ + 128], lhsT=VT[:, vidx, :], rhs=PT[s_][:, ti, :],
                                                start=(ti == 0), stop=(ti == nt - 1)), R=[Bvt, Bpt[s_]], W=[BO[s_]])
                for ti in range(nt):
                    OP("pe", lambda e: e.matmul(PS[7][:, 128 * s_:128 * s_ + 128], lhsT=ONES_B, rhs=PT[s_][:, ti, :],
                                                start=(ti == 0), stop=(ti == nt - 1)), R=[Bpt[s_], B_c], W=[BS[s_]])
                OP("dve", lambda e: e.reciprocal(out=RC[s_][:], in_=PS[7][:, 128 * s_:128 * s_ + 128]), R=[BS[s_]], W=[Brc[s_]])
                OP("dve", lambda e: e.tensor_tensor(out=OH[:, qs], in0=PS[6][:, 128 * s_:128 * s_ + 128], in1=RC[s_][:], op=ALU.mult),
                   R=[BO[s_], Brc[s_]], W=[Boh])
            ncols = NTOK if do_ctx else TL
            DMA("sp", mixT.ap()[CONV + h * 128:CONV + (h + 1) * 128, 0:ncols], OH[:, 0:ncols], R=[Boh], W=[Bmix])
        phase_end()

    def phase_E(l, do_ctx):
        phase_begin()
        NKT = NLT + NCT
        KT = sb("E_KT", [128, NTOK], BF16)
        VT = sb("E_VT", [128, NKT, 128], BF16)
        QT = sb("E_QT", [128, NTOK], BF16)
        OH = sb("E_OH", [128, NTOK], BF16)
        PT = [sb("E_PT%d" % i, [128, 512], BF16) for i in range(3)]
        R1 = sb("E_R1", [128, 512], F32)
        R2 = sb("E_R2", [128, 512], F32)
        T1 = sb("E_T1", [128, 512], F32)
        T2 = sb("E_T2", [128, 512], F32)
        SQ = sb("E_SQ", [128, 512], F32)
        RS = sb("E_RS", [128, 512], F32)
        Bkt, Bvt, Bqt, Boh, Bmix, Br1, Br2, Bt1, Bt2, Bsq, Brs = (Buf() for _ in range(11))
        Bpt = [Buf(), Buf(), Buf()]
        pcnt = 0
        for h in range(NH):
            rows = slice(h * 128, (h + 1) * 128)
            DMA("sp", KT[:, 0:TL], kdf_l.ap()[rows, :], W=[Bkt])
            DMA("sp", KT[:, TL:], kdf_c.ap()[rows, :], W=[Bkt])
            DMA("sp", VT[:, 0:NLT, :], vdf_l.ap()[:, rows].rearrange("(t p) e -> p t e", p=128), W=[Bvt])
            DMA("sp", VT[:, NLT:, :], vdf_c.ap()[:, rows].rearrange("(t p) e -> p t e", p=128), W=[Bvt])
            DMA("sp", QT[:], qdfT.ap()[rows, :], W=[Bqt])
            for gi, (t0, N) in enumerate(groups):
                isctx = gi == C.NG
                if isctx and not do_ctx:
                    continue
                kts = list(range(NLT, NKT)) if isctx else list(range(NCT + NLT))
                for c in range(2):
                    ps_o, ps_s = 2 + c, 4 + c
                    pr = slice(64 * c, 64 * c + 64)
                    for n_i, kt in enumerate(kts):
                        sbk = pcnt % 2
                        pj = pcnt % 3
                        pcnt += 1
                        OP("pe", lambda e: e.matmul(PS[sbk][:, 0:N], lhsT=KT[pr, 128 * kt:128 * kt + 128], rhs=QT[pr, t0:t0 + N], start=True, stop=True),
                           R=[Bkt, Bqt], W=[PB[sbk]])
                        OP("act", lambda e: e.activation(out=PT[pj][:, 0:N], in_=PS[sbk][:, 0:N], func=AF.Exp), R=[PB[sbk]], W=[Bpt[pj]])
                        OP("pe", lambda e: e.matmul(PS[ps_o][:, 0:N], lhsT=VT[:, kt, :], rhs=PT[pj][:, 0:N], start=(n_i == 0), stop=(n_i == len(kts) - 1)),
                           R=[Bvt, Bpt[pj]], W=[PB[ps_o]])
                        OP("pe", lambda e: e.matmul(PS[ps_s][:, 0:N], lhsT=ONES_B, rhs=PT[pj][:, 0:N], start=(n_i == 0), stop=(n_i == len(kts) - 1)),
                           R=[Bpt[pj], B_c], W=[PB[ps_s]])
                OP("dve", lambda e: e.reciprocal(out=R1[:, 0:N], in_=PS[4][:, 0:N]), R=[PB[4]], W=[Br1])
                OP("dve", lambda e: e.reciprocal(out=R2[:, 0:N], in_=PS[5][:, 0:N]), R=[PB[5]], W=[Br2])
                OP("dve", lambda e: e.tensor_tensor(out=T1[:, 0:N], in0=PS[2][:, 0:N], in1=R1[:, 0:N], op=ALU.mult), R=[PB[2], Br1], W=[Bt1])
                OP("dve", lambda e: e.tensor_tensor(out=T2[:, 0:N], in0=PS[3][:, 0:N], in1=R2[:, 0:N], op=ALU.mult), R=[PB[3], Br2], W=[Bt2])
                OP("dve", lambda e: e.scalar_tensor_tensor(out=T1[:, 0:N], in0=T2[:, 0:N], scalar=lamv[:, 0:1], in1=T1[:, 0:N], op0=ALU.mult, op1=ALU.add),
                   R=[Bt1, Bt2, B_lp], W=[Bt1])
                OP("act", lambda e: e.activation(out=SQ[:, 0:N], in_=T1[:, 0:N], func=AF.Square), R=[Bt1], W=[Bsq])
                OP("pe", lambda e: e.matmul(PS[6][:, 0:N], lhsT=ONES_F, rhs=SQ[:, 0:N], start=True, stop=True), R=[Bsq, B_c], W=[PB[6]])
                rsqrt_ps(RS[:, 0:N], Brs, PS[6][:, 0:N], PB[6], 1.0 / 128)
                OP("dve", lambda e: e.scalar_tensor_tensor(out=OH[:, t0:t0 + N], in0=T1[:, 0:N], scalar=gains[:, 4:5], in1=RS[:, 0:N], op0=ALU.mult, op1=ALU.mult),
                   R=[Bt1, Brs, B_lp], W=[Boh])
            ncols = NTOK if do_ctx else TL
            DMA("sp", mixT.ap()[CONV + NAW + h * 128:CONV + NAW + (h + 1) * 128, 0:ncols], OH[:, 0:ncols], R=[Boh], W=[Bmix])
        phase_end()

    def phase_F(l, do_ctx):
        phase_begin()
        X = sb("F_X", [128, KD, 512], F32)
        MIX = sb("F_M", [128, KD, 512], BF16)
        WB = [sb("F_W%d" % i, [128, KD, 256], BF16) for i in range(2)]
        SQ = [sb("F_SQ%d" % i, [128, 512], F32) for i in range(2)]
        TMP = [sb("F_T%d" % i, [128, 512], F32) for i in range(2)]
        RS = sb("F_RS", [128, 512], F32)
        WR = sb("F_WR", [128, KD, 16], BF16)
        EX = sb("F_EX", [16, 512], F32)
        RC = sb("F_RC", [16, 512], F32)
        Bx, Bm, Brs, Bwr, Bex, Brc, Bh2, Baf = (Buf() for _ in range(8))
        Bw = [Buf(), Buf()]
        Bsq = [Buf(), Buf()]
        Btmp = [Buf(), Buf()]
        wo = wbf["w_out"].ap().rearrange("(k p) n -> p k n", p=128)
        DMA("sp", WR[:], wbf["w_router"].ap().rearrange("(k p) n -> p k n", p=128), R=[B_w["w_router"]], W=[Bwr])
        wcnt = 0
        for gi, (t0, N) in enumerate(groups):
            isctx = gi == C.NG
            if isctx and not do_ctx:
                continue
            sidx = 1 if isctx else 0
            DMA("sp", X[:, :, 0:N], xT.ap()[:, t0:t0 + N].rearrange("(k p) t -> p k t", p=128), R=[B_x[gi]], W=[Bx])
            DMA("sp", MIX[:, :, 0:N], mixT.ap()[:, t0:t0 + N].rearrange("(k p) t -> p k t", p=128), W=[Bm])
            for cb in range(D // 256):
                j = wcnt % 2
                wcnt += 1
                DMA("sp", WB[j][:], wo[:, :, cb * 256:(cb + 1) * 256], R=[B_w["w_out"]], W=[Bw[j]])
                for ci in range(2):
                    c = cb * 2 + ci
                    pi = c % 2
                    for k in range(KD):
                        OP("pe", lambda e: e.matmul(PS[pi][:, 0:N], lhsT=WB[j][:, k, ci * 128:(ci + 1) * 128], rhs=MIX[:, k, 0:N],
                                                    start=(k == 0), stop=(k == KD - 1)), R=[Bm, Bw[j]], W=[PB[pi]])
                    OP("dve", lambda e: e.scalar_tensor_tensor(out=X[:, c, 0:N], in0=PS[pi][:, 0:N], scalar=mT[:, 2 * KD + c, sidx:sidx + 1],
                                                               in1=X[:, c, 0:N], op0=ALU.mult, op1=ALU.add), R=[PB[pi], Bx, B_m], W=[Bx])
            DMA("sp", xT.ap()[:, t0:t0 + N].rearrange("(k p) t -> p k t", p=128), X[:, :, 0:N], R=[Bx], W=[B_x[gi]])
            norm_mod(X, MIX, N, sidx, 4, 3, SQ, RS, TMP, Bx, Bm, Bsq, Brs, Btmp, 7)
            DMA("sp", h2T.ap()[:, t0:t0 + N].rearrange("(k p) t -> p k t", p=128), MIX[:, :, 0:N], R=[Bm], W=[Bh2])
            for k in range(KD):
                OP("pe", lambda e: e.matmul(PS[2][0:16, 0:N], lhsT=WR[:, k, :], rhs=MIX[:, k, 0:N], start=(k == 0), stop=(k == KD - 1)),
                   R=[Bm, Bwr], W=[PB[2]])
            OP("act", lambda e: e.activation(out=EX[:, 0:N], in_=PS[2][0:16, 0:N], func=AF.Exp), R=[PB[2]], W=[Bex])
            OP("pe", lambda e: e.matmul(PS[3][0:16, 0:N], lhsT=ONES_F[0:16, 0:16], rhs=EX[:, 0:N], start=True, stop=True), R=[Bex, B_c], W=[PB[3]])
            OP("dve", lambda e: e.reciprocal(out=RC[:, 0:N], in_=PS[3][0:16, 0:N]), R=[PB[3]], W=[Brc])
            OP("dve", lambda e: e.tensor_tensor(out=EX[:, 0:N], in0=EX[:, 0:N], in1=RC[:, 0:N], op=ALU.mult), R=[Bex, Brc], W=[Bex])
            if isctx:
                DMA("sp", aff_c.ap(), EX[:, 0:N], R=[Bex], W=[Baf])
            else:
                DMA("sp", aff_l.ap()[:, t0:t0 + N], EX[:, 0:N], R=[Bex], W=[Baf])
        phase_end()

    def phase_G(l, do_ctx):
        phase_begin()
        A = sb("G_A", [16, TL], F32)
        J = sb("G_J", [16, TL], F32)
        S_ = sb("G_S", [16, 8], F32)
        Ba, Bj, Bs = Buf(), Buf(), Buf()
        sets = [(aff_l, TL, C.capL, 0)] + ([(aff_c, LC, C.capC, 1)] if do_ctx else [])
        for src, n, cap, col in sets:
            DMA("sp", A[:, 0:n], src.ap(), W=[Ba])
            OP("dve", lambda e: e.memset(S_[:, 0:1], 0.0), W=[Bs])
            OP("dve", lambda e: e.memset(S_[:, 1:2], 1.0), R=[Bs], W=[Bs])
            for itn in range(30):
                OP("dve", lambda e: e.tensor_scalar(out=S_[:, 2:3], in0=S_[:, 0:1], scalar1=S_[:, 1:2], scalar2=0.5, op0=ALU.add, op1=ALU.mult), R=[Bs], W=[Bs])
                OP("dve", lambda e: e.tensor_scalar(out=J[:, 0:n], in0=A[:, 0:n], scalar1=S_[:, 2:3], scalar2=None, op0=ALU.is_ge), R=[Ba, Bs], W=[Bj])
                OP("dve", lambda e: e.reduce_sum(out=S_[:, 3:4], in_=J[:, 0:n], axis=mybir.AxisListType.X), R=[Bj, Bs], W=[Bs])
                OP("dve", lambda e: e.tensor_scalar(out=S_[:, 4:5], in0=S_[:, 3:4], scalar1=float(cap) - 0.5, scalar2=None, op0=ALU.is_ge), R=[Bs], W=[Bs])
                OP("dve", lambda e: e.tensor_tensor(out=S_[:, 5:6], in0=S_[:, 2:3], in1=S_[:, 0:1], op=ALU.subtract), R=[Bs], W=[Bs])
                OP("dve", lambda e: e.scalar_tensor_tensor(out=S_[:, 0:1], in0=S_[:, 5:6], scalar=S_[:, 4:5], in1=S_[:, 0:1], op0=ALU.mult, op1=ALU.add), R=[Bs], W=[Bs])
                OP("dve", lambda e: e.tensor_tensor(out=S_[:, 5:6], in0=S_[:, 1:2], in1=S_[:, 2:3], op=ALU.subtract), R=[Bs], W=[Bs])
                OP("dve", lambda e: e.scalar_tensor_tensor(out=S_[:, 1:2], in0=S_[:, 5:6], scalar=S_[:, 4:5], in1=S_[:, 2:3], op0=ALU.mult, op1=ALU.add), R=[Bs], W=[Bs])
            OP("dve", lambda e: e.tensor_copy(out=THR[:, col:col + 1], in_=S_[:, 0:1]), R=[Bs], W=[B_thr])
        phase_end()

    def phase_H(l, do_ctx):
        phase_begin()
        FC, HF = C.FC, C.HF
        HH = HF // 2
        FB = min(256, C.FF)
        NFB = C.FF // FB
        H2 = sb("H_H2", [128, KD, 512], BF16)
        HID = sb("H_HID", [128, HH, 512], BF16)
        WG = [sb("H_WG%d" % i, [128, KD, FB], BF16) for i in range(2)]
        WU = [sb("H_WU%d" % i, [128, KD, FB], BF16) for i in range(2)]
        WD = [sb("H_WD%d" % i, [128, HH, 128], BF16) for i in range(2)]
        AFg = sb("H_AF", [16, 512], F32)
        WGT = sb("H_WGT", [16, 512], BF16)
        SG = [sb("H_SG%d" % i, [128, 512], F32) for i in range(2)]
        TT = [sb("H_TT%d" % i, [128, 512], F32) for i in range(2)]
        XC = [sb("H_XC%d" % i, [128, 512], F32) for i in range(3)]
        Bh2, Bhid, Baf, Bwgt = Buf(), Buf(), Buf(), Buf()
        Bwg, Bwu, Bwd, Bsg, Btt = ([Buf(), Buf()] for _ in range(5))
        Bxc = [Buf(), Buf(), Buf()]
        wg = wbf["w_gate"].ap().rearrange("(e k p) f -> e p k f", p=128, k=KD)
        wu = wbf["w_up"].ap().rearrange("(e k p) f -> e p k f", p=128, k=KD)
        wd = wbf["w_down"].ap().rearrange("(f p) n -> p f n", p=128)
        cw = cdn = cx = cs = 0
        for gi, (t0, N) in enumerate(groups):
            isctx = gi == C.NG
            if isctx and not do_ctx:
                continue
            sidx = 1 if isctx else 0
            DMA("sp", H2[:, :, 0:N], h2T.ap()[:, t0:t0 + N].rearrange("(k p) t -> p k t", p=128), W=[Bh2])
            DMA("sp", AFg[:, 0:N], (aff_c.ap() if isctx else aff_l.ap()[:, t0:t0 + N]), W=[Baf])
            OP("dve", lambda e: e.scalar_tensor_tensor(out=WGT[:, 0:N], in0=AFg[:, 0:N], scalar=THR[:, sidx:sidx + 1], in1=AFg[:, 0:N],
                                                       op0=ALU.is_ge, op1=ALU.mult), R=[Baf, B_thr], W=[Bwgt])
            for half in range(2):
                for el in range(8):
                    ex = half * 8 + el
                    OP("pe", lambda e: e.matmul(PS[6][:, 0:N], lhsT=selb[:, ex, :], rhs=WGT[:, 0:N], start=True, stop=True), R=[Bwgt, B_c], W=[PB[6]])
                    for fb in range(NFB):
                        j = cw % 2
                        cw += 1
                        DMA("sp", WG[j][:], wg[ex][:, :, fb * FB:(fb + 1) * FB], R=[B_w["w_gate"]], W=[Bwg[j]])
                        DMA("sp", WU[j][:], wu[ex][:, :, fb * FB:(fb + 1) * FB], R=[B_w["w_up"]], W=[Bwu[j]])
                        for f2 in range(FB // 128):
                            pg, pu = 2 * (cs % 2), 2 * (cs % 2) + 1
                            sj = cs % 2
                            cs += 1
                            for k in range(KD):
                                OP("pe", lambda e: e.matmul(PS[pg][:, 0:N], lhsT=WG[j][:, k, f2 * 128:(f2 + 1) * 128], rhs=H2[:, k, 0:N],
                                                            start=(k == 0), stop=(k == KD - 1)), R=[Bh2, Bwg[j]], W=[PB[pg]])
                            for k in range(KD):
                                OP("pe", lambda e: e.matmul(PS[pu][:, 0:N], lhsT=WU[j][:, k, f2 * 128:(f2 + 1) * 128], rhs=H2[:, k, 0:N],
                                                            start=(k == 0), stop=(k == KD - 1)), R=[Bh2, Bwu[j]], W=[PB[pu]])
                            OP("act", lambda e: e.activation(out=SG[sj][:, 0:N], in_=PS[pg][:, 0:N], func=AF.Silu), R=[PB[pg]], W=[Bsg[sj]])
                            OP("dve", lambda e: e.tensor_tensor(out=TT[sj][:, 0:N], in0=PS[pu][:, 0:N], in1=SG[sj][:, 0:N], op=ALU.mult),
                               R=[PB[pu], Bsg[sj]], W=[Btt[sj]])
                            hidx = el * FC + fb * (FB // 128) + f2
                            OP("dve", lambda e: e.tensor_tensor(out=HID[:, hidx, 0:N], in0=TT[sj][:, 0:N], in1=PS[6][:, 0:N], op=ALU.mult),
                               R=[Btt[sj], PB[6]], W=[Bhid])
                for cb in range(KD):
                    j = cdn % 2
                    cdn += 1
                    DMA("sp", WD[j][:], wd[:, half * HH:(half + 1) * HH, cb * 128:(cb + 1) * 128], R=[B_w["w_down"]], W=[Bwd[j]])
                    for ci in range(1):
                        c = cb
                        pi = 4 + c % 2
                        xj = cx % 3
                        cx += 1
                        DMA("sp", XC[xj][:, 0:N], xT.ap()[c * 128:(c + 1) * 128, t0:t0 + N], R=[B_x[gi]], W=[Bxc[xj]])
                        for f in range(HH):
                            OP("pe", lambda e: e.matmul(PS[pi][:, 0:N], lhsT=WD[j][:, f, ci * 128:(ci + 1) * 128], rhs=HID[:, f, 0:N],
                                                        start=(f == 0), stop=(f == HH - 1)), R=[Bhid, Bwd[j]], W=[PB[pi]])
                        OP("dve", lambda e: e.scalar_tensor_tensor(out=XC[xj][:, 0:N], in0=PS[pi][:, 0:N], scalar=mT[:, 5 * KD + c, sidx:sidx + 1],
                                                                   in1=XC[xj][:, 0:N], op0=ALU.mult, op1=ALU.add), R=[PB[pi], Bxc[xj], B_m], W=[Bxc[xj]])
                        DMA("sp", xT.ap()[c * 128:(c + 1) * 128, t0:t0 + N], XC[xj][:, 0:N], R=[Bxc[xj]], W=[B_x[gi]], sb=Bxc[xj])
        phase_end()

    def run_all():
        for l in range(L):
            do_ctx = True if C.per_layer else (l < C.DEPTH - 1)
            phase_weights(l)
            phase_mod(l)
            phase_A(l)
            phase_C(l, do_ctx)
            phase_D(l, do_ctx)
            phase_E(l, do_ctx)
            phase_F(l, do_ctx)
            phase_G(l, do_ctx)
            phase_H(l, do_ctx)
        bo = Buf()
        for gi, (t0, N) in enumerate(groups):
            for k in range(KD):
                DMA("sp", outT.ap()[k * 128:(k + 1) * 128, t0:t0 + N], xT.ap()[k * 128:(k + 1) * 128, t0:t0 + N], R=[B_x[gi]], W=[bo])
        tr.barrier()

    def dbg_out(name, t):
        phase_begin()
        o = nc.dram_tensor("dbg_" + name, list(t.shape), F32, kind="ExternalOutput")
        bb = Buf()
        if t.dtype == F32:
            DMA("sp", o.ap(), t.ap(), W=[bb])
        else:
            rows, cols = t.shape
            a = sb("dbg_a_" + name, [128, cols], BF16)
            b2 = sb("dbg_b_" + name, [128, cols], F32)
            Ba, Bb = Buf(), Buf()
            for r0 in range(0, rows, 128):
                DMA("sp", a[:], t.ap()[r0:r0 + 128, :], W=[Ba])
                OP("dve", lambda e: e.tensor_copy(out=b2[:], in_=a[:]), R=[Ba], W=[Bb])
                DMA("sp", o.ap()[r0:r0 + 128, :], b2[:], R=[Bb], W=[bb])
        phase_end()

    return nc, tr, locals()


def _consts():
    c = np.zeros((128, 5, 128), np.float32)
    c[:, 0, :] = 1.0
    c[0:64, 1, 0:64] = 1.0
    c[64:128, 1, 64:128] = 1.0
    c[:, 2, :] = np.eye(128, dtype=np.float32)
    for p in range(128):
        d = p % 32
        partner = p + 16 if d < 16 else p - 16
        c[partner, 3, p] = 1.0
    sel = np.zeros((16, 16, 128), np.float32)
    for e in range(16):
        sel[e, e, :] = 1.0
    return c, sel


def _rope_tables(cfg, quarter):
    C = cfg
    nf = 16
    inv = (10000.0 ** (-np.arange(nf, dtype=np.float32) / nf)).astype(np.float32)
    t_idx = np.arange(quarter * C.TL, (quarter + 1) * C.TL)
    ang_r = (t_idx // 64).astype(np.float32)[:, None] * inv
    ang_c = (t_idx % 64).astype(np.float32)[:, None] * inv
    tab = np.zeros((128, 2, C.NTOK), np.float32)
    tab[:, 0, :] = 1.0
    for p in range(128):
        d = p % 64
        ang = ang_r if d < 32 else ang_c
        i = d % 16
        sgn = -1.0 if (d % 32) < 16 else 1.0
        tab[p, 0, :C.TL] = np.cos(ang[:, i])
        tab[p, 1, :C.TL] = sgn * np.sin(ang[:, i])
    return tab


def prep_inputs(cfg, inp, nlayers=None, NSH=8, layer0=0, xT_prev=None):
    C = cfg
    L = C.DEPTH if nlayers is None else nlayers
    D, KD, TL, LC = C.D, C.KD, C.TL, C.LC
    consts, sel = _consts()
    f = lambda a: np.ascontiguousarray(a, dtype=np.float32)
    wfull = dict(w_in=inp["w_in"], w_out=inp["w_out"], conv_pw=inp["conv_pw"], ada_down=inp["ada_down"],
                 ada_up=inp["ada_up"], w_router=inp["w_router"],
                 w_gate=inp["w_gate"].reshape(inp["w_gate"].shape[0], 16 * D, C.FF),
                 w_up=inp["w_up"].reshape(inp["w_up"].shape[0], 16 * D, C.FF),
                 w_down=inp["w_down"].reshape(inp["w_down"].shape[0], 16 * C.FF, D))
    rpb = inp["na_rpb"]
    qrow = np.arange(128) // 64
    qcol = np.arange(128) % 64
    cs = np.clip(qcol - 8, 0, 64 - 16)
    nab = np.full((rpb.shape[0], C.NH, 9, 128, 128), NEG, np.float32)
    for t in range(9):
        krow = (-8 + 2 * t) + (np.arange(128) // 64)
        kcol = np.arange(128) % 64
        dr = krow[None, :] - qrow[:, None]
        dc = kcol[None, :] - qcol[:, None]
        ok = (kcol[None, :] >= cs[:, None]) & (kcol[None, :] < cs[:, None] + 16) & (np.abs(dr) <= 7)
        dri = np.clip(dr + 7, 0, 14)
        dci = np.clip(dc + 15, 0, 30)
        vals = rpb[:, :, dri, dci]
        nab[:, :, t] = np.where(ok[None, None], vals, NEG)
    wfull["nab"] = nab.reshape(rpb.shape[0], C.NH * 9 * 128, 128)
    abias = f(inp["ada_bias"][:L].reshape(L, 6 * KD, 128).transpose(0, 2, 1))
    convp = np.zeros((L, 128, C.CC, 34), np.float32)
    convp[:, :, :, 0:31] = inp["conv_dw"][:L].reshape(L, 31, C.CC, 128).transpose(0, 3, 2, 1)
    convp[:, :, :, 31] = inp["conv_db"][:L].reshape(L, C.CC, 128).transpose(0, 2, 1)
    convp[:, :, :, 32] = inp["conv_ln_g"][:L].reshape(L, C.CC, 128).transpose(0, 2, 1)
    convp[:, :, :, 33] = inp["conv_ln_b"][:L].reshape(L, C.CC, 128).transpose(0, 2, 1)
    gains = np.zeros((L, 128, 8), np.float32)
    lam = np.zeros((L, 1, 256), np.float32)
    for l in range(L):
        lam_init = 0.8 - 0.6 * math.exp(-0.3 * (l + layer0))
        gains[l, :, 0] = inp["na_q_gain"][l]
        gains[l, :, 1] = inp["na_k_gain"][l]
        gains[l, :, 2] = inp["diff_q_gain"][l].reshape(128)
        gains[l, :, 3] = inp["diff_k_gain"][l].reshape(128)
        gains[l, :, 4] = inp["diff_out_gain"][l]
        gains[l, :, 5] = lam_init
        gains[l, :, 6] = 128.0 ** -0.5
        gains[l, :, 7] = 1.0 - lam_init
        lam[l, 0, :] = inp["diff_lam"][l].reshape(256)
    in_maps = []
    ncore = 8 if C.split == 4 else 2
    for core in range(ncore):
        b, qt = (core // 4, core % 4) if C.split == 4 else (core, 0)
        m = {}
        if xT_prev is not None:
            m["xT"] = xT_prev[core]
        else:
            xl = inp["x"][b, qt * TL:(qt + 1) * TL, :]
            m["xT"] = f(np.concatenate([xl, inp["ctx"][b]], axis=0).T)
        cT = np.zeros((128, KD, 2), np.float32)
        cT[:, :, 0] = inp["c"][b].reshape(KD, 128).T
        cT[:, :, 1] = inp["c_ctx"].reshape(KD, 128).T
        m["cT"] = cT
        for k, w in wfull.items():
            K = w.shape[1]
            m[k] = f(w[:L]) if NSH == 1 else f(w[:L, core * (K // 8):(core + 1) * (K // 8), :])
        m["abias"] = abias
        m["convp"] = convp
        m["gains"] = gains
        m["lam"] = lam
        m["rope"] = _rope_tables(C, qt)
        m["consts"] = consts
        m["sel"] = sel
        rm = np.zeros((128, 5, 9, 128), np.float32)
        npair = C.RL // 2
        for ci, pi in enumerate((0, 1, 2, npair - 2, npair - 1)):
            r0 = qt * C.RL + 2 * pi
            for t in range(9):
                krow = r0 - 8 + 2 * t + (np.arange(128) // 64)
                qr = r0 + (np.arange(128) // 64)
                rs = np.clip(qr - 4, 0, C.ROWS - 8)
                ok = (krow[None, :] >= rs[:, None]) & (krow[None, :] < rs[:, None] + 8)
                rm[:, ci, t, :] = np.where(ok, 0.0, NEG)
        m["rmask"] = rm
        sp = np.zeros((128, 8), np.float32)
        if qt > 0:
            sp[:, qt - 1] = 1.0
        if qt < 3:
            sp[:, 4 + qt + 1] = 1.0
        m["selpn"] = sp
        in_maps.append(m)
    return in_maps


PER_LAYER_KEYS = ("ada_down", "ada_up", "ada_bias", "w_in", "conv_dw", "conv_db", "conv_ln_g", "conv_ln_b", "conv_pw",
                  "na_q_gain", "na_k_gain", "na_rpb", "diff_q_gain", "diff_k_gain", "diff_lam", "diff_out_gain", "w_out",
                  "w_router", "w_gate", "w_up", "w_down")


def kernel(**inputs):
    cfg = Cfg(split=1)
    cfg.per_layer = True
    inp = {k: np.asarray(v) for k, v in inputs.items()}
    nc, tr, loc = build(cfg, nlayers=1, NSH=1)
    loc["run_all"]()
    xprev = None
    for l in range(cfg.DEPTH):
        inp_l = {k: (v[l:l + 1] if k in PER_LAYER_KEYS else v) for k, v in inp.items()}
        in_maps = prep_inputs(cfg, inp_l, nlayers=1, NSH=1, layer0=l, xT_prev=xprev)
        res = run_bass_kernel_spmd(nc, in_maps, core_ids=[0, 1])
        xprev = [np.ascontiguousarray(res.results[b]["outT"], dtype=np.float32) for b in range(2)]
        del in_maps, res
    out = np.stack([xprev[b][:, :cfg.TL].T for b in range(2)], 0)
    return np.ascontiguousarray(out, dtype=np.float32)
```
